# Optimizing a Trainium2 kernel written in Bass

```python
import math, functools
import jax, jax.numpy as jnp
from jax import lax
import numpy as np


D_MODEL = 2048
BATCH = 1
SEQ = 16384
DEPTH = 4

GRID_W = 64
CTX_LEN = 256
EPS = 1e-6
ROPE_THETA = 10000.0
Q_BLOCK = 128
ROT_DIM = 64
MLA_HEADS = 8
MLA_NOPE = 128
MLA_ROPE = 64
MLA_V = 128
MLA_Q_RANK = 512
MLA_KV_RANK = 256
DIFF_HEADS = 4
DIFF_QK = 64
DIFF_V = 128
NA_HEADS = 4
NA_DIM = 128
NA_KH = 8
NA_KW = 16
NA_QCB = 16
NA_KCS = 32
N_EXPERTS = 32
TOP_K = 4
D_EXPERT = 768
SWIGLU_ALPHA = 1.702
SWIGLU_LIMIT = 7.0
EXPERT_BLOCK = 128

MIX_WIDTH = MLA_HEADS * MLA_V + DIFF_HEADS * DIFF_V + NA_HEADS * NA_DIM
IN_SPLITS = (MLA_Q_RANK, MLA_KV_RANK, MLA_ROPE,
             DIFF_HEADS * 2 * DIFF_QK, DIFF_HEADS * 2 * DIFF_QK, DIFF_HEADS * DIFF_V,
             NA_HEADS * NA_DIM, NA_HEADS * NA_DIM, NA_HEADS * NA_DIM)
IN_WIDTH = sum(IN_SPLITS)
F32 = jnp.float32

kernel_name = 'hybrid_mla_diff_natten_moe_dit'


def rmsnorm(x, g):
    xf = x.astype(F32)
    y = xf * lax.rsqrt(jnp.mean(xf * xf, axis=-1, keepdims=True) + EPS)
    return (y * g.astype(F32)).astype(x.dtype)


def modulate(h, shift, scale):
    return h * (1 + scale) + shift


def axial_rope_tables(n_lat, n_ctx):
    quarter = ROT_DIM // 4
    inv = 1.0 / (ROPE_THETA ** (jnp.arange(quarter, dtype=F32) / quarter))
    t = jnp.arange(n_lat)
    row = (t // GRID_W).astype(F32)
    col = (t % GRID_W).astype(F32)
    ang = jnp.concatenate([row[:, None] * inv, col[:, None] * inv], axis=-1)
    ang = jnp.concatenate([ang, jnp.zeros((n_ctx, ROT_DIM // 2), F32)], axis=0)
    return jnp.cos(ang), jnp.sin(ang)


def apply_rope(x, cos, sin):
    half = x.shape[-1] // 2
    expand = (1,) * (x.ndim - 3)
    cos = cos.reshape((cos.shape[0],) + expand + (half,))
    sin = sin.reshape((sin.shape[0],) + expand + (half,))
    xf = x.astype(F32)
    x1, x2 = xf[..., :half], xf[..., half:]
    return jnp.concatenate([x1 * cos - x2 * sin, x1 * sin + x2 * cos], axis=-1).astype(x.dtype)


def map_query_blocks(fn, q):
    B, N = q.shape[:2]
    nb = N // Q_BLOCK
    qb = jnp.moveaxis(q.reshape((B, nb, Q_BLOCK) + q.shape[2:]), 1, 0)
    o = lax.map(fn, qb)
    return jnp.moveaxis(o, 0, 1).reshape((B, N) + o.shape[3:])


def softmax_attention(q, k, v, scale):
    s = jnp.einsum('bqhd,bkhd->bhqk', q, k).astype(F32) * scale
    p = jax.nn.softmax(s, axis=-1).astype(v.dtype)
    return jnp.einsum('bhqk,bkhd->bqhd', p, v)


def differential_attention(q, k, v, lam, scale):
    s = jnp.einsum('bqhtd,bkhtd->bthqk', q, k).astype(F32) * scale
    p = jax.nn.softmax(s, axis=-1)
    w = (p[:, 0] - lam * p[:, 1]).astype(v.dtype)
    return jnp.einsum('bhqk,bkhd->bqhd', w, v)


def column_window_tables():
    ncb = GRID_W // NA_QCB
    j = np.arange(ncb)
    cs = np.clip(j * NA_QCB - NA_KW // 2, 0, GRID_W - NA_KCS)
    col_idx = cs[:, None] + np.arange(NA_KCS)
    qc = j[:, None] * NA_QCB + np.arange(NA_QCB)
    wc = np.clip(qc - NA_KW // 2, 0, GRID_W - NA_KW)[..., None]
    kc = col_idx[:, None, :]
    mask = (kc >= wc) & (kc < wc + NA_KW)
    rel = np.clip(kc - qc[..., None] + NA_KW - 1, 0, 2 * NA_KW - 2)
    return col_idx, mask, rel


def neighborhood_attention(q, k, v, k_ctx, v_ctx, rpb, scale):
    B, N, H, dh = q.shape
    rows = N // GRID_W
    kh = min(NA_KH, rows)
    col_idx, col_mask, col_rel = column_window_tables()
    ncb = GRID_W // NA_QCB
    qg = q.reshape(B, rows, GRID_W, H, dh)
    kg = k.reshape(B, rows, GRID_W, H, dh)
    vg = v.reshape(B, rows, GRID_W, H, dh)
    rpb_f = rpb.astype(F32)
    mask = jnp.asarray(col_mask)[None, None, :, :, None, :]
    n_loc = kh * NA_KCS

    def one_row(r):
        rs = jnp.clip(r - kh // 2, 0, rows - kh)
        q_r = lax.dynamic_index_in_dim(qg, r, axis=1, keepdims=False).reshape(B, ncb, NA_QCB, H, dh)
        k_b = lax.dynamic_slice_in_dim(kg, rs, kh, axis=1)[:, :, col_idx]
        v_b = lax.dynamic_slice_in_dim(vg, rs, kh, axis=1)[:, :, col_idx]
        s_loc = jnp.einsum('bjqhd,brjchd->bhjqrc', q_r, k_b).astype(F32) * scale
        row_rel = rs + jnp.arange(kh) - r + (NA_KH - 1)
        bias = rpb_f[:, row_rel][:, :, col_rel].transpose(0, 2, 3, 1, 4)
        s_loc = jnp.where(mask, s_loc + bias[None], -jnp.inf).reshape(B, H, ncb, NA_QCB, n_loc)
        s_ctx = jnp.einsum('bjqhd,bkhd->bhjqk', q_r, k_ctx).astype(F32) * scale
        p = jax.nn.softmax(jnp.concatenate([s_loc, s_ctx], axis=-1), axis=-1).astype(v.dtype)
        p_loc = p[..., :n_loc].reshape(B, H, ncb, NA_QCB, kh, NA_KCS)
        o = (jnp.einsum('bhjqrc,brjchd->bjqhd', p_loc, v_b)
             + jnp.einsum('bhjqk,bkhd->bjqhd', p[..., n_loc:], v_ctx))
        return o.reshape(B, GRID_W, H, dh)

    o = lax.map(one_row, jnp.arange(rows))
    return jnp.moveaxis(o, 0, 1).reshape(B, N, H, dh)


def hybrid_mixer(h, hc, cos, sin, w_in, mla_qa_norm, w_uq, mla_kva_norm, w_ukv, mla_q_gain,
                 mla_knope_gain, mla_kpe_gain, diff_q_gain, diff_k_gain, diff_lambda, diff_subln,
                 na_q_gain, na_k_gain, na_rpb, w_out, lam_init, with_ctx_out):
    B, N, _ = h.shape
    T = N + hc.shape[1]
    proj = jnp.concatenate([h, hc], axis=1) @ w_in
    split_at = np.cumsum(IN_SPLITS)[:-1].tolist()
    qa, kva, kpe, dq, dk, dv, nq, nk, nv = jnp.split(proj, split_at, axis=-1)

    q_a = (rmsnorm(qa, mla_qa_norm) @ w_uq).reshape(B, T, MLA_HEADS, MLA_NOPE + MLA_ROPE)
    q_a = rmsnorm(q_a, mla_q_gain)
    q_a = jnp.concatenate([q_a[..., :MLA_NOPE], apply_rope(q_a[..., MLA_NOPE:], cos, sin)], axis=-1)
    kv = (rmsnorm(kva, mla_kva_norm) @ w_ukv).reshape(B, T, MLA_HEADS, MLA_NOPE + MLA_V)
    k_nope = rmsnorm(kv[..., :MLA_NOPE], mla_knope_gain)
    v_a = kv[..., MLA_NOPE:]
    k_pe = apply_rope(rmsnorm(kpe, mla_kpe_gain), cos, sin)
    k_a = jnp.concatenate([k_nope, jnp.broadcast_to(k_pe[:, :, None], (B, T, MLA_HEADS, MLA_ROPE))], axis=-1)
    mla_scale = (MLA_NOPE + MLA_ROPE) ** -0.5
    o_a = map_query_blocks(lambda qi: softmax_attention(qi, k_a, v_a, mla_scale), q_a[:, :N])

    q_b = apply_rope(rmsnorm(dq.reshape(B, T, DIFF_HEADS, 2, DIFF_QK), diff_q_gain), cos, sin)
    k_b = apply_rope(rmsnorm(dk.reshape(B, T, DIFF_HEADS, 2, DIFF_QK), diff_k_gain), cos, sin)
    v_b = dv.reshape(B, T, DIFF_HEADS, DIFF_V)
    lf = diff_lambda.astype(F32)
    lam = jnp.exp(jnp.sum(lf[0] * lf[1])) - jnp.exp(jnp.sum(lf[2] * lf[3])) + lam_init
    diff_scale = DIFF_QK ** -0.5
    o_b = map_query_blocks(lambda qi: differential_attention(qi, k_b, v_b, lam, diff_scale), q_b[:, :N])
    o_b = rmsnorm(o_b, diff_subln) * (1 - lam_init)

    q_c = rmsnorm(nq.reshape(B, T, NA_HEADS, NA_DIM), na_q_gain)
    k_c = rmsnorm(nk.reshape(B, T, NA_HEADS, NA_DIM), na_k_gain)
    v_c = nv.reshape(B, T, NA_HEADS, NA_DIM)
    na_scale = NA_DIM ** -0.5
    o_c = neighborhood_attention(q_c[:, :N], k_c[:, :N], v_c[:, :N], k_c[:, N:], v_c[:, N:], na_rpb, na_scale)

    o = jnp.concatenate([o_a.reshape(B, N, -1), o_b.reshape(B, N, -1), o_c.reshape(B, N, -1)], axis=-1) @ w_out
    if not with_ctx_out:
        return o, None
    oc_a = softmax_attention(q_a[:, N:], k_a[:, N:], v_a[:, N:], mla_scale)
    oc_b = rmsnorm(differential_attention(q_b[:, N:], k_b[:, N:], v_b[:, N:], lam, diff_scale), diff_subln) * (1 - lam_init)
    oc_c = softmax_attention(q_c[:, N:], k_c[:, N:], v_c[:, N:], na_scale)
    C = T - N
    oc = jnp.concatenate([oc_a.reshape(B, C, -1), oc_b.reshape(B, C, -1), oc_c.reshape(B, C, -1)], axis=-1) @ w_out
    return o, oc


def moe_ffn(h, router_w, router_b, w_gu, b_gu, w_down, b_down):
    shp = h.shape
    xf = h.reshape(-1, shp[-1])
    n = xf.shape[0]
    logits = (xf @ router_w).astype(F32) + router_b.astype(F32)
    top_v, top_e = lax.top_k(logits, TOP_K)
    gates = jax.nn.softmax(top_v, axis=-1)
    n_assign = n * TOP_K
    flat_e = top_e.reshape(-1)
    flat_tok = jnp.arange(n_assign, dtype=jnp.int32) // TOP_K
    order = jnp.argsort(flat_e)
    e_sorted = flat_e[order]
    counts = jnp.bincount(flat_e, length=N_EXPERTS)
    padded = (counts + EXPERT_BLOCK - 1) // EXPERT_BLOCK * EXPERT_BLOCK
    start = jnp.cumsum(counts) - counts
    pad_end = jnp.cumsum(padded)
    pad_start = pad_end - padded
    dest = pad_start[e_sorted] + jnp.arange(n_assign, dtype=jnp.int32) - start[e_sorted]
    n_blocks = -(-n_assign // EXPERT_BLOCK) + N_EXPERTS
    cap = n_blocks * EXPERT_BLOCK
    row_tok = jnp.full((cap,), n, jnp.int32).at[dest].set(flat_tok[order])
    row_gate = jnp.zeros((cap,), F32).at[dest].set(gates.reshape(-1)[order])
    blk_e = jnp.minimum(jnp.searchsorted(pad_end, jnp.arange(n_blocks) * EXPERT_BLOCK, side='right'), N_EXPERTS - 1)
    xpad = jnp.concatenate([xf, jnp.zeros((1, xf.shape[1]), xf.dtype)], axis=0)
    xb = xpad[row_tok].reshape(n_blocks, EXPERT_BLOCK, xf.shape[1])

    def expert_block(args):
        xi, e = args
        gu = xi @ w_gu[e] + b_gu[e]
        glu = jnp.minimum(gu[..., ::2], SWIGLU_LIMIT)
        lin = jnp.clip(gu[..., 1::2], -SWIGLU_LIMIT, SWIGLU_LIMIT)
        act = glu * jax.nn.sigmoid(SWIGLU_ALPHA * glu) * (lin + 1)
        return act @ w_down[e] + b_down[e]

    yb = lax.map(expert_block, (xb, blk_e)).reshape(cap, -1)
    y = jax.ops.segment_sum(yb * row_gate[:, None].astype(yb.dtype), row_tok, num_segments=n + 1)[:n]
    return y.reshape(shp)


def setup_inputs(seed: int = 0) -> dict:
    key = jax.random.key(seed)
    ks = jax.random.split(key, 32)
    L, D = DEPTH, D_MODEL

    def nrm(k, shape, scale):
        return jax.random.normal(k, shape, F32) * scale

    def gain(k, shape):
        return 1.0 + 0.05 * jax.random.normal(k, shape, F32)

    return {
        'x': nrm(ks[0], (BATCH, SEQ, D), 1.0),
        'c': nrm(ks[1], (BATCH, D), 1.0),
        'ctx': nrm(ks[2], (BATCH, CTX_LEN, D), 1.0),
        'c_ctx': nrm(ks[3], (D,), 1.0),
        'w_ada': nrm(ks[4], (L, D, 6 * D), 0.5 * D ** -0.5),
        'b_ada': nrm(ks[5], (L, 6 * D), 0.01),
        'attn_norm': gain(ks[6], (L, D)),
        'ffn_norm': gain(ks[7], (L, D)),
        'w_in': nrm(ks[8], (L, D, IN_WIDTH), D ** -0.5),
        'mla_qa_norm': gain(ks[9], (L, MLA_Q_RANK)),
        'w_uq': nrm(ks[10], (L, MLA_Q_RANK, MLA_HEADS * (MLA_NOPE + MLA_ROPE)), MLA_Q_RANK ** -0.5),
        'mla_kva_norm': gain(ks[11], (L, MLA_KV_RANK)),
        'w_ukv': nrm(ks[12], (L, MLA_KV_RANK, MLA_HEADS * (MLA_NOPE + MLA_V)), MLA_KV_RANK ** -0.5),
        'mla_q_gain': gain(ks[13], (L, MLA_NOPE + MLA_ROPE)),
        'mla_knope_gain': gain(ks[14], (L, MLA_NOPE)),
        'mla_kpe_gain': gain(ks[15], (L, MLA_ROPE)),
        'diff_q_gain': gain(ks[16], (L, DIFF_QK)),
        'diff_k_gain': gain(ks[17], (L, DIFF_QK)),
        'diff_lambda': nrm(ks[18], (L, 4, DIFF_QK), 0.1),
        'diff_subln': gain(ks[19], (L, DIFF_V)),
        'na_q_gain': gain(ks[20], (L, NA_DIM)),
        'na_k_gain': gain(ks[21], (L, NA_DIM)),
        'na_rpb': nrm(ks[22], (L, NA_HEADS, 2 * NA_KH - 1, 2 * NA_KW - 1), 0.1),
        'w_out': nrm(ks[23], (L, MIX_WIDTH, D), MIX_WIDTH ** -0.5),
        'router_w': nrm(ks[24], (L, D, N_EXPERTS), D ** -0.5),
        'router_b': nrm(ks[25], (L, N_EXPERTS), 0.01),
        'w_gu': nrm(ks[26], (L, N_EXPERTS, D, 2 * D_EXPERT), D ** -0.5),
        'b_gu': nrm(ks[27], (L, N_EXPERTS, 2 * D_EXPERT), 0.01),
        'w_down': nrm(ks[28], (L, N_EXPERTS, D_EXPERT, D), D_EXPERT ** -0.5),
        'b_down': nrm(ks[29], (L, N_EXPERTS, D), 0.01),
    }


def reference(x, c, ctx, c_ctx, w_ada, b_ada, attn_norm, ffn_norm, w_in, mla_qa_norm, w_uq,
              mla_kva_norm, w_ukv, mla_q_gain, mla_knope_gain, mla_kpe_gain, diff_q_gain, diff_k_gain,
              diff_lambda, diff_subln, na_q_gain, na_k_gain, na_rpb, w_out, router_w, router_b,
              w_gu, b_gu, w_down, b_down):
    n_lat = x.shape[1]
    n_ctx = ctx.shape[1]
    cos, sin = axial_rope_tables(n_lat, n_ctx)
    silu_c = jax.nn.silu(c)
    silu_cc = jax.nn.silu(c_ctx)
    xc = ctx
    for l in range(DEPTH):
        with_ctx_out = l < DEPTH - 1
        lam_init = 0.8 - 0.6 * math.exp(-0.3 * l)
        mod = silu_c @ w_ada[l] + b_ada[l]
        mod_c = silu_cc @ w_ada[l] + b_ada[l]
        sh1, sc1, g1, sh2, sc2, g2 = [m[:, None, :] for m in jnp.split(mod, 6, axis=-1)]
        sh1c, sc1c, g1c, sh2c, sc2c, g2c = jnp.split(mod_c, 6, axis=-1)
        h = modulate(rmsnorm(x, attn_norm[l]), sh1, sc1)
        hc = modulate(rmsnorm(xc, attn_norm[l]), sh1c, sc1c)
        o, oc = hybrid_mixer(h, hc, cos, sin, w_in[l], mla_qa_norm[l], w_uq[l], mla_kva_norm[l], w_ukv[l],
                             mla_q_gain[l], mla_knope_gain[l], mla_kpe_gain[l], diff_q_gain[l], diff_k_gain[l],
                             diff_lambda[l], diff_subln[l], na_q_gain[l], na_k_gain[l], na_rpb[l], w_out[l],
                             lam_init, with_ctx_out)
        x = x + g1 * o
        h = modulate(rmsnorm(x, ffn_norm[l]), sh2, sc2)
        if with_ctx_out:
            xc = xc + g1c * oc
            hc = modulate(rmsnorm(xc, ffn_norm[l]), sh2c, sc2c)
            y = moe_ffn(jnp.concatenate([h, hc], axis=1), router_w[l], router_b[l], w_gu[l], b_gu[l], w_down[l], b_down[l])
            x = x + g2 * y[:, :n_lat]
            xc = xc + g2c * y[:, n_lat:]
        else:
            x = x + g2 * moe_ffn(h, router_w[l], router_b[l], w_gu[l], b_gu[l], w_down[l], b_down[l])
    return x
```

```python
import contextlib
import math
import numpy as np
import ml_dtypes
import concourse.bass as bass
import concourse.mybir as mybir
from concourse.bass_utils import run_bass_kernel_spmd

F32 = mybir.dt.float32
BF16 = mybir.dt.bfloat16
ALU = mybir.AluOpType
AF = mybir.ActivationFunctionType
NPBF = ml_dtypes.bfloat16

NCORES = 8
D = 2048
KC = 16
CTX = 256
EPS = 1e-6
GRID_W = 64
SEM_EPOCH = 20000


class Buf:
    __slots__ = ("name", "w", "r")

    def __init__(self, name=""):
        self.name = name
        self.w = []
        self.r = []


class Prog:
    ENGS = ("pe", "act", "dve", "pool", "sp")
    NDMA_SEM = 8

    def __init__(self, nc):
        self.nc = nc
        self.ops = {e: [] for e in self.ENGS}
        self.seen = {e: {x: -1 for x in self.ENGS} for e in self.ENGS}
        self.seen_dma = {e: {} for e in self.ENGS}
        self.ndma = {e: 0 for e in self.ENGS}
        self.dma_ops = {e: [] for e in self.ENGS}

    def _add_wait(self, E, op, dep, raw):
        X, j = dep
        dop = self.ops[X][j]
        if dop["dma"]:
            k = dop["k"]
            key = (X, k % self.NDMA_SEM)
            if self.seen_dma[E].get(key, -1) >= k:
                return
            self.seen_dma[E][key] = k
            op["waits"].append(("dma", X, k))
        else:
            if X == E and not raw:
                return
            if self.seen[E][X] >= j:
                return
            self.seen[E][X] = j
            dop["signal"] = True
            op["waits"].append(("eng", X, j))

    def op(self, eng, fn, reads=(), writes=(), dma=False):
        E = eng
        idx = len(self.ops[E])
        op = {"fn": fn, "waits": [], "signal": False, "dma": dma}
        if dma:
            k = self.ndma[E]
            self.ndma[E] += 1
            op["k"] = k
            self.dma_ops[E].append(idx)
            if k >= self.NDMA_SEM:
                self._add_wait(E, op, (E, self.dma_ops[E][k - self.NDMA_SEM]), True)
        ref = (E, idx)
        for b in reads:
            for d in b.w:
                self._add_wait(E, op, d, True)
        for b in writes:
            for d in b.w:
                self._add_wait(E, op, d, dma)
            for d in b.r:
                self._add_wait(E, op, d, dma)
        for b in reads:
            if not dma:
                b.r = [d for d in b.r if not (d[0] == E and not self.ops[E][d[1]]["dma"])]
            b.r.append(ref)
        for b in writes:
            if b.r:
                b.w = [ref]
                b.r = []
            else:
                if not dma:
                    b.w = [d for d in b.w if not (d[0] == E and not self.ops[E][d[1]]["dma"])]
                b.w.append(ref)
        self.ops[E].append(op)
        return ref

    def emit(self):
        nc = self.nc
        sigval = {}
        nsig = {}
        for E in self.ENGS:
            c = 0
            vals = []
            for o in self.ops[E]:
                if o["signal"] and not o["dma"]:
                    c += 1
                vals.append(c)
            sigval[E] = vals
            nsig[E] = c
        with contextlib.ExitStack() as st:
            csem = {E: [st.enter_context(nc.semaphore(f"c_{E}_{i}")) for i in range(nsig[E] // SEM_EPOCH + 1)]
                    for E in self.ENGS}
            dsem = {E: [st.enter_context(nc.semaphore(f"d_{E}_{i}")) for i in range(self.NDMA_SEM)]
                    for E in self.ENGS if self.ndma[E]}
            block = st.enter_context(nc.Block())
            ND = self.NDMA_SEM

            def gen(E):
                def body(eng):
                    sv = sigval[E]
                    for i, o in enumerate(self.ops[E]):
                        for (kind, X, j) in o["waits"]:
                            if kind == "dma":
                                eng.wait_ge(dsem[X][j % ND], 16 * (j // ND + 1))
                            else:
                                v = sigval[X][j]
                                eng.wait_ge(csem[X][(v - 1) // SEM_EPOCH], (v - 1) % SEM_EPOCH + 1)
                        ins = o["fn"](eng)
                        if o["dma"]:
                            ins.then_inc(dsem[E][o["k"] % ND], 16)
                        elif o["signal"]:
                            ins.then_inc(csem[E][(sv[i] - 1) // SEM_EPOCH], 1)
                return body

            block.tensor(gen("pe"))
            block.scalar(gen("act"))
            block.vector(gen("dve"))
            block.gpsimd(gen("pool"))
            block.sync(gen("sp"))

    def stats(self):
        return {E: (len(self.ops[E]), sum(len(o["waits"]) for o in self.ops[E])) for E in self.ENGS}


class K:
    def __init__(self):
        self.nc = bass.Bass("TRN2", target_bir_lowering=False)
        self.P = Prog(self.nc)
        self.st = contextlib.ExitStack()
        self.out_bufs = []
        self.rr = {}

    def din(self, name, shape, dt=F32):
        return self.nc.dram_tensor(name, list(shape), dt, kind="ExternalInput").ap()

    def dout(self, name, shape, dt=F32):
        ap = self.nc.dram_tensor(name, list(shape), dt, kind="ExternalOutput").ap()
        b = Buf(name)
        self.out_bufs.append(b)
        return ap, b

    def sb(self, name, shape, dt=F32):
        return self.st.enter_context(self.nc.sbuf_tensor("s_" + name, list(shape), dt))

    def ps(self, name, shape, dt=F32):
        return self.st.enter_context(self.nc.psum_tensor("p_" + name, list(shape), dt))

    def dma(self, q, out, in_, reads=(), writes=()):
        return self.P.op(q, lambda e: e.dma_start(out=out, in_=in_), reads, writes, dma=True)

    def mm(self, out, lhsT, rhs, start, stop, reads=(), writes=()):
        return self.P.op("pe", lambda e: e.matmul(out, lhsT=lhsT, rhs=rhs, start=start, stop=stop), reads, writes)

    def act(self, out, in_, func, reads=(), writes=(), bias=0.0, scale=1.0, eng="act"):
        return self.P.op(eng, lambda e: e.activation(out=out, in_=in_, func=func, bias=bias, scale=scale), reads, writes)

    def tt(self, out, in0, in1, op, reads=(), writes=(), eng="dve"):
        return self.P.op(eng, lambda e: e.tensor_tensor(out=out, in0=in0, in1=in1, op=op), reads, writes)

    def ts(self, out, in0, s1, s2, op0, op1=None, reads=(), writes=(), eng="dve"):
        if op1 is None:
            return self.P.op(eng, lambda e: e.tensor_scalar(out=out, in0=in0, scalar1=s1, scalar2=None, op0=op0), reads, writes)
        return self.P.op(eng, lambda e: e.tensor_scalar(out=out, in0=in0, scalar1=s1, scalar2=s2, op0=op0, op1=op1), reads, writes)

    def stt(self, out, in0, scalar, in1, op0, op1, reads=(), writes=(), eng="dve"):
        return self.P.op(eng, lambda e: e.scalar_tensor_tensor(out=out, in0=in0, scalar=scalar, in1=in1, op0=op0, op1=op1),
                         reads, writes)

    def copy(self, out, in_, reads=(), writes=(), eng="dve"):
        return self.P.op(eng, lambda e: e.tensor_copy(out=out, in_=in_), reads, writes)

    def recip(self, out, in_, reads=(), writes=()):
        return self.P.op("dve", lambda e: e.reciprocal(out=out, in_=in_), reads, writes)

    def memset(self, ap, v, writes=(), eng="dve"):
        return self.P.op(eng, lambda e: e.memset(ap, v), (), writes)

    def finish(self):
        self.P.op("sp", lambda e: e.nop(), reads=self.out_bufs)
        self.P.emit()
        self.st.close()
        return self.nc


class Rot:
    def __init__(self, k, name, shape, dt, n, psum=False):
        alloc = k.ps if psum else k.sb
        self.t = [alloc(f"{name}{i}", shape, dt) for i in range(n)]
        self.b = [Buf(f"{name}{i}") for i in range(n)]
        self.i = 0
        self.n = n

    def next(self):
        i = self.i
        self.i = (i + 1) % self.n
        return self.t[i], self.b[i]


def make_masks(k):
    m = {}
    for name in ("ones", "h0", "h1", "bd2"):
        m[name] = (k.sb(f"mask_{name}", [128, 128], BF16), Buf(f"mask_{name}"))
    k.memset(m["ones"][0][:], 1.0, [m["ones"][1]])
    for name in ("h0", "h1", "bd2"):
        k.memset(m[name][0][:], 0.0, [m[name][1]])
    k.memset(m["h0"][0][0:64, :], 1.0, [m["h0"][1]])
    k.memset(m["h1"][0][64:128, :], 1.0, [m["h1"][1]])
    k.memset(m["bd2"][0][0:64, 0:64], 1.0, [m["bd2"][1]])
    k.memset(m["bd2"][0][64:128, 64:128], 1.0, [m["bd2"][1]])
    return m


FM_QA, FM_KVA, FM_KPE, FM_DQ, FM_DK, FM_NQ, FM_NK = 0, 4, 6, 7, 11, 15, 19
N_FM = 23
G_QAN, G_KVAN, G_QN, G_QR, G_KN, G_KPE, G_DQ, G_DK, G_NQ, G_NK = 0, 4, 6, 7, 8, 9, 10, 11, 12, 13
NG = 14
G_FAC = {G_QAN: math.sqrt(512.0), G_QAN + 1: math.sqrt(512.0), G_QAN + 2: math.sqrt(512.0), G_QAN + 3: math.sqrt(512.0),
         G_KVAN: 16.0, G_KVAN + 1: 16.0, G_QN: 1.0, G_QR: 1.0, G_KN: math.sqrt(128.0), G_KPE: 8.0,
         G_DQ: 1.0, G_DK: 8.0, G_NQ: 1.0, G_NK: math.sqrt(128.0)}


def build_A(NL):
    NT = NL + CTX
    blocks = [(i * 512, 512, 0) for i in range(NL // 512)] + [(NL, CTX, 1)]
    k = K()
    nc = k.nc
    xT = k.din("xT", [KC, 128, NT])
    cvec = k.din("cvec", [128, KC, 2])
    w_ada = k.din("w_ada", [96, 128, KC, 128])
    b_ada = k.din("b_ada", [128, 96])
    attn_norm = k.din("attn_norm", [128, KC])
    w_in_fm = k.din("w_in_fm", [N_FM, 128, KC, 128])
    w_in_tm = k.din("w_in_tm", [2, 128, KC, 512])
    w_uq_fm = k.din("w_uq_fm", [12, 128, 4, 128])
    w_ukv_fm = k.din("w_ukv_fm", [8, 128, 2, 128])
    w_ukv_tm = k.din("w_ukv_tm", [128, 2, 1024])
    gvec_d = k.din("gvec", [128, NG])
    cosT_d = k.din("cosT", [128, NT])
    sinS_d = k.din("sinS", [128, NT])
    pmat_d = k.din("pmat", [128, 128])

    QAn, bQAn = k.dout("QAn", [8, 128, NT], BF16)
    QAr, bQAr = k.dout("QAr", [8, 64, NT], BF16)
    KAn, bKAn = k.dout("KAn", [8, 128, NT], BF16)
    KPE, bKPE = k.dout("KPE", [64, NT], BF16)
    VA, bVA = k.dout("VA", [8, 128, NT // 128, 128], BF16)
    QB, bQB = k.dout("QB", [4, 128, NT], BF16)
    KB, bKB = k.dout("KB", [4, 128, NT], BF16)
    VB, bVB = k.dout("VB", [4, 128, NT // 128, 128], BF16)
    QC, bQC = k.dout("QC", [4, 128, NT], BF16)
    KCo, bKC = k.dout("KC", [4, 128, NT], BF16)
    VC, bVC = k.dout("VC", [4, 128, NT // 128, 128], BF16)
    MODo, bMOD = k.dout("modT", [128, 96, 2], F32)

    masks = make_masks(k)
    gvec = k.sb("gvec", [128, NG]); b_gvec = Buf("gvec")
    k.dma("sp", gvec[:], gvec_d, writes=[b_gvec])
    for col, fac in G_FAC.items():
        if fac != 1.0:
            k.ts(gvec[:, col:col + 1], gvec[:, col:col + 1], float(fac), None, ALU.mult, reads=[b_gvec], writes=[b_gvec])
    pmat = k.sb("pmat", [128, 128], BF16); b_pmat = Buf("pmat")
    k.dma("pool", pmat[:], pmat_d, writes=[b_pmat])

    cv = k.sb("cv", [128, KC, 2]); b_cv = Buf("cv")
    cvs = k.sb("cvs", [128, KC, 2], BF16); b_cvs = Buf("cvs")
    k.dma("sp", cv[:], cvec, writes=[b_cv])
    k.act(cvs[:], cv[:], AF.Silu, reads=[b_cv], writes=[b_cvs])
    bada = k.sb("bada", [128, 96]); b_bada = Buf("bada")
    k.dma("sp", bada[:], b_ada, writes=[b_bada])
    modps = k.ps("modps", [128, 96, 2]); b_modps = Buf("modps")
    wada = Rot(k, "wada", [128, KC, 128], BF16, 3)
    for j in range(96):
        wt, wb = wada.next()
        k.dma("pool", wt[:], w_ada[j], writes=[wb])
        for kc in range(KC):
            k.mm(modps[:, j, :], wt[:, kc, :], cvs[:, kc, :], kc == 0, kc == KC - 1, reads=[wb, b_cvs], writes=[b_modps])
    modT = k.sb("modT", [128, 96, 2]); b_modT = Buf("modT")
    for s in range(2):
        k.tt(modT[:, :, s], modps[:, :, s], bada[:], ALU.add, reads=[b_modps, b_bada], writes=[b_modT])
    k.dma("sp", MODo, modT[:], reads=[b_modT], writes=[bMOD])
    an = k.sb("an", [128, KC]); b_an = Buf("an")
    k.dma("sp", an[:], attn_norm, writes=[b_an])
    A1 = k.sb("A1", [128, KC, 2]); b_A1 = Buf("A1")
    for s in range(2):
        k.ts(A1[:, :, s], modT[:, 16:32, s], 1.0, math.sqrt(float(D)), ALU.add, ALU.mult, reads=[b_modT], writes=[b_A1])
        k.tt(A1[:, :, s], A1[:, :, s], an[:], ALU.mult, reads=[b_A1, b_an], writes=[b_A1])

    xb = k.sb("xb", [128, KC, 512]); b_xb = Buf("xb")
    sq = Rot(k, "sq", [128, 512], BF16, 4)
    tmpf = Rot(k, "tmpf", [128, 512], F32, 3)
    hT = k.sb("hT", [128, KC, 512], BF16); b_hT = [Buf(f"hT{c}") for c in range(KC)]
    wg = Rot(k, "wg", [128, 4, KC, 128], BF16, 2)
    raw = Rot(k, "raw", [128, 512], F32, 6)
    qan = k.sb("qan", [128, 4, 512], BF16); b_qan = [Buf(f"qan{c}") for c in range(4)]
    kvan = k.sb("kvan", [128, 2, 512], BF16); b_kvan = [Buf(f"kvan{c}") for c in range(2)]
    cs = k.sb("cs", [128, 2, 512]); b_cs = Buf("cs")
    wuq = Rot(k, "wuq", [128, 3, 4, 128], BF16, 2)
    wukv = k.sb("wukv", [128, 8, 2, 128], BF16); b_wukv = Buf("wukv")
    wukvv = k.sb("wukvv", [128, 2, 1024], BF16); b_wukvv = Buf("wukvv")
    wv = Rot(k, "wv", [128, KC, 512], BF16, 1)
    ybf = Rot(k, "ybf", [128, 512], BF16, 3)
    obf = Rot(k, "obf", [128, 512], BF16, 4)
    sqs = Rot(k, "sqs", [128, 512], F32, 2)
    rstd = Rot(k, "rstd", [128, 512], F32, 3)
    pp = Rot(k, "pp", [128, 512], F32, 3, psum=True)
    pss = Rot(k, "pss", [128, 512], F32, 2, psum=True)
    prot = Rot(k, "prot", [128, 512], F32, 1, psum=True)

    def rms(chunks, Dn, n, np_=128):
        pt, pb = pss.next()
        for i, (rap, rbuf, mname) in enumerate(chunks):
            st_, sb_ = sq.next()
            k.act(st_[0:np_, 0:n], rap, AF.Square, reads=[rbuf], writes=[sb_])
            mt, mb = masks[mname]
            k.mm(pt[0:np_, 0:n], mt[0:np_, 0:np_], st_[0:np_, 0:n], i == 0, i == len(chunks) - 1, reads=[mb, sb_], writes=[pb])
        s_t, s_b = sqs.next()
        k.act(s_t[0:np_, 0:n], pt[0:np_, 0:n], AF.Sqrt, reads=[pb], writes=[s_b], bias=float(Dn * EPS))
        r_t, r_b = rstd.next()
        k.recip(r_t[0:np_, 0:n], s_t[0:np_, 0:n], reads=[s_b], writes=[r_b])
        return r_t, r_b

    def rope(y_t, y_b, n, np_=128):
        rt, rb = prot.next()
        k.mm(rt[0:np_, 0:n], pmat[0:np_, 0:np_], y_t[0:np_, 0:n], True, True, reads=[b_pmat, y_b], writes=[rb])
        t1, t1b = tmpf.next()
        k.tt(t1[0:np_, 0:n], y_t[0:np_, 0:n], cs[0:np_, 0, 0:n], ALU.mult, reads=[y_b, b_cs], writes=[t1b])
        t2, t2b = tmpf.next()
        k.tt(t2[0:np_, 0:n], rt[0:np_, 0:n], cs[0:np_, 1, 0:n], ALU.mult, reads=[rb, b_cs], writes=[t2b])
        o_t, o_b = obf.next()
        k.tt(o_t[0:np_, 0:n], t1[0:np_, 0:n], t2[0:np_, 0:n], ALU.add, reads=[t1b, t2b], writes=[o_b], eng="pool")
        return o_t, o_b

    def proj_chunk(wt, wb, ci, rhs_t, rhs_bufs, nk, n):
        pt, pb = pp.next()
        for kc in range(nk):
            k.mm(pt[:, 0:n], wt[:, ci, kc, :], rhs_t[:, kc, 0:n], kc == 0, kc == nk - 1, reads=[wb, rhs_bufs[kc]], writes=[pb])
        return pt, pb

    def evac_raw(pt, pb, n):
        r_t, r_b = raw.next()
        k.copy(r_t[:, 0:n], pt[:, 0:n], reads=[pb], writes=[r_b])
        return r_t, r_b

    def load_wg(c0, nchunk):
        wt, wb = wg.next()
        k.dma("pool", wt[:, 0:nchunk], w_in_fm[c0:c0 + nchunk].rearrange("c p k m -> p c k m"), writes=[wb])
        return wt, wb

    k.dma("pool", wukv[:], w_ukv_fm.rearrange("c p k m -> p c k m"), writes=[b_wukv])
    k.dma("pool", wukvv[:], w_ukv_tm, writes=[b_wukvv])

    for (t0, n, is_ctx) in blocks:
        ntile = n // 128
        k.dma("sp", xb[:, :, 0:n], xT[:, :, t0:t0 + n].rearrange("c p t -> p c t"), writes=[b_xb])
        k.dma("sp", cs[:, 0, 0:n], cosT_d[:, t0:t0 + n], writes=[b_cs])
        k.dma("sp", cs[:, 1, 0:n], sinS_d[:, t0:t0 + n], writes=[b_cs])
        r_t, r_b = rms([(xb[:, c, 0:n], b_xb, "ones") for c in range(KC)], D, n)
        for c in range(KC):
            t1, t1b = tmpf.next()
            k.stt(t1[:, 0:n], xb[:, c, 0:n], A1[:, c, is_ctx:is_ctx + 1], r_t[:, 0:n], ALU.mult, ALU.mult,
                  reads=[b_xb, b_A1, r_b], writes=[t1b])
            k.act(hT[:, c, 0:n], t1[:, 0:n], AF.Identity, reads=[t1b, b_modT], writes=[b_hT[c]],
                  bias=modT[:, c, is_ctx:is_ctx + 1])

        wt, wb = load_wg(FM_QA, 4)
        rws = []
        for ci in range(4):
            pt, pb = proj_chunk(wt, wb, ci, hT, b_hT, KC, n)
            rws.append(evac_raw(pt, pb, n))
        r_t, r_b = rms([(rw[0][:, 0:n], rw[1], "ones") for rw in rws], 512, n)
        for ci in range(4):
            k.stt(qan[:, ci, 0:n], rws[ci][0][:, 0:n], gvec[:, G_QAN + ci:G_QAN + ci + 1], r_t[:, 0:n], ALU.mult, ALU.mult,
                  reads=[rws[ci][1], b_gvec, r_b], writes=[b_qan[ci]])
        wt, wb = load_wg(FM_KVA, 2)
        rws = []
        for ci in range(2):
            pt, pb = proj_chunk(wt, wb, ci, hT, b_hT, KC, n)
            rws.append(evac_raw(pt, pb, n))
        r_t, r_b = rms([(rw[0][:, 0:n], rw[1], "ones") for rw in rws], 256, n)
        for ci in range(2):
            k.stt(kvan[:, ci, 0:n], rws[ci][0][:, 0:n], gvec[:, G_KVAN + ci:G_KVAN + ci + 1], r_t[:, 0:n], ALU.mult, ALU.mult,
                  reads=[rws[ci][1], b_gvec, r_b], writes=[b_kvan[ci]])
        for j in range(4):
            wt, wb = wuq.next()
            k.dma("pool", wt[:, 0:2], w_uq_fm[2 * j:2 * j + 2].rearrange("c p k m -> p c k m"), writes=[wb])
            k.dma("pool", wt[:, 2], w_uq_fm[8 + j], writes=[wb])
            rws = []
            for ci in range(3):
                pt, pb = pp.next()
                for kc in range(4):
                    k.mm(pt[:, 0:n], wt[:, ci, kc, :], qan[:, kc, 0:n], kc == 0, kc == 3, reads=[wb, b_qan[kc]], writes=[pb])
                rws.append(evac_raw(pt, pb, n))
            y_t, y_b = ybf.next()
            for hh in range(2):
                h = 2 * j + hh
                r_t, r_b = rms([(rws[hh][0][:, 0:n], rws[hh][1], "ones"), (rws[2][0][:, 0:n], rws[2][1], "h%d" % hh)], 192, n)
                o_t, o_b = obf.next()
                k.stt(o_t[:, 0:n], rws[hh][0][:, 0:n], gvec[:, G_QN:G_QN + 1], r_t[:, 0:n], ALU.mult, ALU.mult,
                      reads=[rws[hh][1], b_gvec, r_b], writes=[o_b])
                k.dma("sp", QAn[h, :, t0:t0 + n], o_t[:, 0:n], reads=[o_b], writes=[bQAn])
                sl = slice(64 * hh, 64 * hh + 64)
                k.stt(y_t[sl, 0:n], rws[2][0][sl, 0:n], gvec[sl, G_QR:G_QR + 1], r_t[sl, 0:n], ALU.mult, ALU.mult,
                      reads=[rws[2][1], b_gvec, r_b], writes=[y_b])
            o_t, o_b = rope(y_t, y_b, n)
            for hh in range(2):
                k.dma("sp", QAr[2 * j + hh, :, t0:t0 + n], o_t[64 * hh:64 * hh + 64, 0:n], reads=[o_b], writes=[bQAr])
        for h in range(8):
            pt, pb = pp.next()
            for kc in range(2):
                k.mm(pt[:, 0:n], wukv[:, h, kc, :], kvan[:, kc, 0:n], kc == 0, kc == 1, reads=[b_wukv, b_kvan[kc]], writes=[pb])
            r_t, r_b = rms([(pt[:, 0:n], pb, "ones")], 128, n)
            o_t, o_b = obf.next()
            k.stt(o_t[:, 0:n], pt[:, 0:n], gvec[:, G_KN:G_KN + 1], r_t[:, 0:n], ALU.mult, ALU.mult,
                  reads=[pb, b_gvec, r_b], writes=[o_b])
            k.dma("sp", KAn[h, :, t0:t0 + n], o_t[:, 0:n], reads=[o_b], writes=[bKAn])
        for tt in range(ntile):
            for half in range(2):
                pt, pb = pp.next()
                for kc in range(2):
                    k.mm(pt[:, :], kvan[:, kc, tt * 128:(tt + 1) * 128], wukvv[:, kc, half * 512:(half + 1) * 512],
                         kc == 0, kc == 1, reads=[b_kvan[kc], b_wukvv], writes=[pb])
                o_t, o_b = obf.next()
                k.act(o_t[:, :], pt[:, :], AF.Copy, reads=[pb], writes=[o_b])
                k.dma("sp", VA[half * 4:half * 4 + 4, :, (t0 // 128) + tt, :].rearrange("h p d -> p h d"),
                      o_t[:, :].rearrange("p (h d) -> p h d", h=4), reads=[o_b], writes=[bVA])
        wt, wb = load_wg(FM_KPE, 1)
        pt, pb = proj_chunk(wt, wb, 0, hT, b_hT, KC, n)
        r_t, r_b = rms([(pt[0:64, 0:n], pb, "ones")], 64, n, np_=64)
        y_t, y_b = ybf.next()
        k.stt(y_t[0:64, 0:n], pt[0:64, 0:n], gvec[0:64, G_KPE:G_KPE + 1], r_t[0:64, 0:n], ALU.mult, ALU.mult,
              reads=[pb, b_gvec, r_b], writes=[y_b])
        o_t, o_b = rope(y_t, y_b, n, np_=64)
        k.dma("sp", KPE[:, t0:t0 + n], o_t[0:64, 0:n], reads=[o_b], writes=[bKPE])
        for (c0, gcol, dst, bdst, mname, Dn, roped) in ((FM_DQ, G_DQ, QB, bQB, "bd2", 64, True), (FM_DK, G_DK, KB, bKB, "bd2", 64, True),
                                                       (FM_NQ, G_NQ, QC, bQC, "ones", 128, False), (FM_NK, G_NK, KCo, bKC, "ones", 128, False)):
            wt, wb = load_wg(c0, 4)
            for ci in range(4):
                pt, pb = proj_chunk(wt, wb, ci, hT, b_hT, KC, n)
                r_t, r_b = rms([(pt[:, 0:n], pb, mname)], Dn, n)
                if roped:
                    y_t, y_b = ybf.next()
                    k.stt(y_t[:, 0:n], pt[:, 0:n], gvec[:, gcol:gcol + 1], r_t[:, 0:n], ALU.mult, ALU.mult,
                          reads=[pb, b_gvec, r_b], writes=[y_b])
                    o_t, o_b = rope(y_t, y_b, n)
                else:
                    o_t, o_b = obf.next()
                    k.stt(o_t[:, 0:n], pt[:, 0:n], gvec[:, gcol:gcol + 1], r_t[:, 0:n], ALU.mult, ALU.mult,
                          reads=[pb, b_gvec, r_b], writes=[o_b])
                k.dma("sp", dst[ci, :, t0:t0 + n], o_t[:, 0:n], reads=[o_b], writes=[bdst])
        for gi, (dst, bdst) in enumerate(((VB, bVB), (VC, bVC))):
            wt, wb = wv.next()
            k.dma("pool", wt[:], w_in_tm[gi], writes=[wb])
            for tt in range(ntile):
                pt, pb = pp.next()
                for kc in range(KC):
                    k.mm(pt[:, :], hT[:, kc, tt * 128:(tt + 1) * 128], wt[:, kc, :], kc == 0, kc == KC - 1,
                         reads=[b_hT[kc], wb], writes=[pb])
                o_t, o_b = obf.next()
                k.act(o_t[:, :], pt[:, :], AF.Copy, reads=[pb], writes=[o_b])
                k.dma("sp", dst[:, :, (t0 // 128) + tt, :].rearrange("h p d -> p h d"),
                      o_t[:, :].rearrange("p (h d) -> p h d", h=4), reads=[o_b], writes=[bdst])
    print("A stats", k.P.stats())
    return k.finish()


def fm_tiles(w, cols):
    K_ = w.shape[0]
    sub = w[:, cols]
    ncb = sub.shape[1] // 128
    return np.ascontiguousarray(sub.reshape(K_ // 128, 128, ncb, 128).transpose(2, 1, 0, 3))


def tm_tiles(w, cols):
    K_ = w.shape[0]
    sub = w[:, cols]
    return np.ascontiguousarray(sub.reshape(K_ // 128, 128, sub.shape[1]).transpose(1, 0, 2))


def vec_fm(v):
    return np.ascontiguousarray(v.reshape(-1, 128).T)


IN_OFF = dict(qa=0, kva=512, kpe=768, dq=832, dk=1344, dv=1856, nq=2368, nk=2880, nv=3392)


def rope_tables(pos_lat, n_ctx):
    quarter = 16
    inv = (1.0 / (10000.0 ** (np.arange(quarter, dtype=np.float32) / np.float32(quarter)))).astype(np.float32)
    row = (pos_lat // GRID_W).astype(np.float32)
    col = (pos_lat % GRID_W).astype(np.float32)
    ang = np.concatenate([row[:, None] * inv, col[:, None] * inv], axis=-1).astype(np.float32)
    ang = np.concatenate([ang, np.zeros((n_ctx, 32), np.float32)], axis=0)
    cos = np.cos(ang).astype(np.float32).T
    sin = np.sin(ang).astype(np.float32).T
    cosT = np.concatenate([cos, cos, cos, cos], axis=0)
    sinS = np.concatenate([-sin, sin, -sin, sin], axis=0)
    return np.ascontiguousarray(cosT), np.ascontiguousarray(sinS)


def perm_matrix():
    pm = np.zeros((128, 128), np.float32)
    for m in range(128):
        kk = m + 32 if (m % 64) < 32 else m - 32
        pm[kk, m] = 1.0
    return pm


def prep_A_layer(inp, l):
    w_in = inp["w_in"][l]
    fm_cols = np.concatenate([
        np.arange(IN_OFF["qa"], IN_OFF["qa"] + 512), np.arange(IN_OFF["kva"], IN_OFF["kva"] + 256),
        np.arange(IN_OFF["kpe"], IN_OFF["kpe"] + 64), np.arange(IN_OFF["kpe"], IN_OFF["kpe"] + 64),
        np.arange(IN_OFF["dq"], IN_OFF["dq"] + 512), np.arange(IN_OFF["dk"], IN_OFF["dk"] + 512),
        np.arange(IN_OFF["nq"], IN_OFF["nq"] + 512), np.arange(IN_OFF["nk"], IN_OFF["nk"] + 512)])
    w_uq = inp["w_uq"][l]
    uq_cols = np.concatenate([np.arange(h * 192, h * 192 + 128) for h in range(8)] +
                             [np.arange(h * 192 + 128, h * 192 + 192) for h in range(8)])
    w_ukv = inp["w_ukv"][l]
    ukv_n = np.concatenate([np.arange(h * 256, h * 256 + 128) for h in range(8)])
    ukv_v = np.concatenate([np.arange(h * 256 + 128, h * 256 + 256) for h in range(8)])
    t2 = lambda v: np.concatenate([v, v])
    gv = np.zeros((128, NG), np.float32)
    gv[:, G_QAN:G_QAN + 4] = vec_fm(inp["mla_qa_norm"][l])
    gv[:, G_KVAN:G_KVAN + 2] = vec_fm(inp["mla_kva_norm"][l])
    gv[:, G_QN] = inp["mla_q_gain"][l][:128]
    gv[:, G_QR] = t2(inp["mla_q_gain"][l][128:])
    gv[:, G_KN] = inp["mla_knope_gain"][l]
    gv[:, G_KPE] = t2(inp["mla_kpe_gain"][l])
    gv[:, G_DQ] = t2(inp["diff_q_gain"][l])
    gv[:, G_DK] = t2(inp["diff_k_gain"][l])
    gv[:, G_NQ] = inp["na_q_gain"][l]
    gv[:, G_NK] = inp["na_k_gain"][l]
    return dict(
        w_ada=fm_tiles(inp["w_ada"][l], np.arange(6 * D)),
        b_ada=vec_fm(inp["b_ada"][l]),
        attn_norm=vec_fm(inp["attn_norm"][l]),
        w_in_fm=fm_tiles(w_in, fm_cols),
        w_in_tm=np.stack([tm_tiles(w_in, np.arange(IN_OFF["dv"], IN_OFF["dv"] + 512)),
                          tm_tiles(w_in, np.arange(IN_OFF["nv"], IN_OFF["nv"] + 512))]),
        w_uq_fm=fm_tiles(w_uq, uq_cols),
        w_ukv_fm=fm_tiles(w_ukv, ukv_n),
        w_ukv_tm=tm_tiles(w_ukv, ukv_v),
        gvec=gv,
        pmat=perm_matrix(),
    )


def build_B(NL, SEQ, debug=False):
    NT = NL + CTX
    NK = SEQ + CTX
    R = NL // 64
    HT = (R + 16) * 64
    lat_blocks = [(i * 512, 512) for i in range(NL // 512)]
    ctx_blk = (NL, CTX)
    kchunks = [(i * 16, 16) for i in range(SEQ // 2048)] + [(SEQ // 128, 2)]
    ctx_chunk = [kchunks[-1]]
    k = K()
    QAn = k.din("QAn", [8, 128, NT], BF16)
    QAr = k.din("QAr", [8, 64, NT], BF16)
    QB = k.din("QB", [4, 128, NT], BF16)
    QC = k.din("QC", [4, 128, NT], BF16)
    KAn = k.din("KAn_all", [8, 128, NK], BF16)
    KPE = k.din("KPE_all", [64, NK], BF16)
    VA = k.din("VA_all", [8, 128, NK // 128, 128], BF16)
    KB = k.din("KB_all", [4, 128, NK], BF16)
    VB = k.din("VB_all", [4, 128, NK // 128, 128], BF16)
    KCh = k.din("KC_h", [4, 128, HT], BF16)
    VCh = k.din("VC_h", [4, 128, HT // 128, 128], BF16)
    KCc = k.din("KC_ctx", [4, 128, CTX], BF16)
    VCc = k.din("VC_ctx", [4, 128, 2, 128], BF16)
    nabias = k.din("nabias", [4, R, 128, 512])
    w_out = k.din("w_out_fm", [16, 128, KC, 128])
    modT_d = k.din("modT", [128, 96, 2])
    xT = k.din("xT", [KC, 128, NT])
    dlam_d = k.din("dlam", [128, 256])
    subln_d = k.din("subln", [128, 1])
    lamc_d = k.din("lamc", [128, 2])
    x1T, b_x1T = k.dout("x1T", [KC, 128, NT])

    masks = make_masks(k)
    ones_t, ones_b = masks["ones"]
    modT = k.sb("modT", [128, 96, 2]); b_modT = Buf("modT")
    k.dma("sp", modT[:], modT_d, writes=[b_modT])
    dl = k.sb("dl", [128, 256]); b_dl = Buf("dl")
    k.dma("sp", dl[:], dlam_d, writes=[b_dl])
    lamc = k.sb("lamc", [128, 2]); b_lamc = Buf("lamc")
    k.dma("sp", lamc[:], lamc_d, writes=[b_lamc])
    subln = k.sb("subln", [128, 1]); b_subln = Buf("subln")
    k.dma("sp", subln[:], subln_d, writes=[b_subln])
    pr = k.sb("pr", [128, 128]); b_pr = Buf("pr")
    sm = k.sb("sm", [128, 4]); b_sm = Buf("sm")
    k.tt(pr[:, 0:64], dl[:, 0:64], dl[:, 64:128], ALU.mult, reads=[b_dl], writes=[b_pr])
    k.tt(pr[:, 64:128], dl[:, 128:192], dl[:, 192:256], ALU.mult, reads=[b_dl], writes=[b_pr])
    for i in range(2):
        k.P.op("dve", lambda e, i=i: e.reduce_sum(out=sm[:, i:i + 1], in_=pr[:, 64 * i:64 * i + 64], axis=mybir.AxisListType.X),
               reads=[b_pr], writes=[b_sm])
    k.act(sm[:, 2:4], sm[:, 0:2], AF.Exp, reads=[b_sm], writes=[b_sm])
    neglam = k.sb("neglam", [128, 1]); b_neglam = Buf("neglam")
    k.tt(neglam[:], sm[:, 3:4], sm[:, 2:3], ALU.subtract, reads=[b_sm], writes=[b_neglam])
    k.tt(neglam[:], neglam[:], lamc[:, 0:1], ALU.subtract, reads=[b_neglam, b_lamc], writes=[b_neglam])
    k.ts(subln[:], subln[:], lamc[:, 1:2], math.sqrt(128.0), ALU.mult, ALU.mult, reads=[b_subln, b_lamc], writes=[b_subln])

    attnT = k.sb("attnT", [128, 16, NT], BF16); b_attnT = [Buf(f"attnT{c}") for c in range(16)]
    qn = Rot(k, "qn", [128, NT], BF16, 2)
    qr = Rot(k, "qr", [64, NT], BF16, 2)
    kbuf = Rot(k, "kbuf", [128, 2048], BF16, 3)
    kpebuf = Rot(k, "kpebuf", [64, 2048], BF16, 3)
    vbuf = Rot(k, "vbuf", [128, 16, 128], BF16, 3)
    et = Rot(k, "et", [128, 512], BF16, 6)
    accs = Rot(k, "acc", [128, 512], F32, 4)
    hi = Rot(k, "hi", [128, 512], BF16, 2)
    lo = Rot(k, "lo", [128, 512], BF16, 2)
    rec = Rot(k, "rec", [128, 512], F32, 3)
    of = Rot(k, "of", [128, 512], F32, 4)
    sqb = Rot(k, "sqb", [128, 512], BF16, 2)
    ps_o = Rot(k, "pso", [128, 512], F32, 4, psum=True)
    ps_s = Rot(k, "pss", [128, 512], F32, 2, psum=True)
    ps_m = Rot(k, "psm", [128, 512], F32, 2, psum=True)

    def attn(vqs, chunks, load_chunk):
        first = True
        for ci, (t0, nt) in enumerate(chunks):
            kps, (v_t, v_b) = load_chunk(t0, nt)
            for kt in range(nt):
                last = (ci == len(chunks) - 1 and kt == nt - 1)
                for vq in vqs:
                    n = vq["n"]
                    s_t, s_b = ps_s.next()
                    npc = len(vq["pieces"])
                    for pi, (qap, qbuf, kpi, psl) in enumerate(vq["pieces"]):
                        kt_t, kt_b = kps[kpi]
                        k.mm(s_t[:, 0:n], kt_t[psl, kt * 128:(kt + 1) * 128], qap, pi == 0, pi == npc - 1,
                             reads=[kt_b, qbuf], writes=[s_b])
                    e_t, e_b = et.next()
                    k.act(e_t[:, 0:n], s_t[:, 0:n], AF.Exp, reads=[s_b], writes=[e_b])
                    o_t, o_b = vq["O"]
                    k.mm(o_t[:, 0:n], v_t[:, kt, :], e_t[:, 0:n], first, last, reads=[v_b, e_b], writes=[o_b])
                    a_t, a_b = vq["acc"]
                    if first:
                        k.copy(a_t[:, 0:n], e_t[:, 0:n], reads=[e_b], writes=[a_b], eng=vq["eng"])
                    else:
                        k.tt(a_t[:, 0:n], a_t[:, 0:n], e_t[:, 0:n], ALU.add, reads=[a_b, e_b], writes=[a_b], eng=vq["eng"])
                first = False

    def recip_of_sum(vq):
        n = vq["n"]
        a_t, a_b = vq["acc"]
        h_t, h_b = hi.next()
        k.act(h_t[:, 0:n], a_t[:, 0:n], AF.Copy, reads=[a_b], writes=[h_b])
        l_t, l_b = lo.next()
        k.tt(l_t[:, 0:n], a_t[:, 0:n], h_t[:, 0:n], ALU.subtract, reads=[a_b, h_b], writes=[l_b])
        m_t, m_b = ps_m.next()
        k.mm(m_t[:, 0:n], ones_t[:], h_t[:, 0:n], True, False, reads=[ones_b, h_b], writes=[m_b])
        k.mm(m_t[:, 0:n], ones_t[:], l_t[:, 0:n], False, True, reads=[ones_b, l_b], writes=[m_b])
        r_t, r_b = rec.next()
        k.recip(r_t[:, 0:n], m_t[:, 0:n], reads=[m_b], writes=[r_b])
        return r_t, r_b

    def fin_plain(vq, chunk, q0):
        n = vq["n"]
        r_t, r_b = recip_of_sum(vq)
        o_t, o_b = vq["O"]
        k.tt(attnT[:, chunk, q0:q0 + n], o_t[:, 0:n], r_t[:, 0:n], ALU.mult, reads=[o_b, r_b], writes=[b_attnT[chunk]])

    def fin_diff(vq0, vq1, chunk, q0):
        n = vq0["n"]
        r0, rb0 = recip_of_sum(vq0)
        r1, rb1 = recip_of_sum(vq1)
        a_t, a_b = of.next()
        k.tt(a_t[:, 0:n], vq0["O"][0][:, 0:n], r0[:, 0:n], ALU.mult, reads=[vq0["O"][1], rb0], writes=[a_b])
        c_t, c_b = of.next()
        k.tt(c_t[:, 0:n], vq1["O"][0][:, 0:n], r1[:, 0:n], ALU.mult, reads=[vq1["O"][1], rb1], writes=[c_b])
        d_t, d_b = of.next()
        k.stt(d_t[:, 0:n], c_t[:, 0:n], neglam[:, 0:1], a_t[:, 0:n], ALU.mult, ALU.add, reads=[c_b, b_neglam, a_b], writes=[d_b])
        s_t, s_b = sqb.next()
        k.act(s_t[:, 0:n], d_t[:, 0:n], AF.Square, reads=[d_b], writes=[s_b])
        m_t, m_b = ps_m.next()
        k.mm(m_t[:, 0:n], ones_t[:], s_t[:, 0:n], True, True, reads=[ones_b, s_b], writes=[m_b])
        q_t, q_b = of.next()
        k.act(q_t[:, 0:n], m_t[:, 0:n], AF.Sqrt, reads=[m_b], writes=[q_b], bias=float(128 * EPS))
        r_t, r_b = rec.next()
        k.recip(r_t[:, 0:n], q_t[:, 0:n], reads=[q_b], writes=[r_b])
        k.stt(attnT[:, chunk, q0:q0 + n], d_t[:, 0:n], subln[:, 0:1], r_t[:, 0:n], ALU.mult, ALU.mult,
              reads=[d_b, b_subln, r_b], writes=[b_attnT[chunk]])

    engs = ["dve", "pool", "dve", "pool"]

    for h in range(8):
        qn_t, qn_b = qn.next()
        qr_t, qr_b = qr.next()
        k.dma("sp", qn_t[:], QAn[h], writes=[qn_b])
        k.dma("sp", qr_t[:], QAr[h], writes=[qr_b])

        def load_mla(t0, nt, h=h):
            kt_t, kt_b = kbuf.next()
            kp_t, kp_b = kpebuf.next()
            v_t, v_b = vbuf.next()
            k.dma("sp", kt_t[:, 0:nt * 128], KAn[h, :, t0 * 128:(t0 + nt) * 128], writes=[kt_b])
            k.dma("sp", kp_t[:, 0:nt * 128], KPE[:, t0 * 128:(t0 + nt) * 128], writes=[kp_b])
            k.dma("sp", v_t[:, 0:nt, :], VA[h, :, t0:t0 + nt, :], writes=[v_b])
            return [(kt_t, kt_b), (kp_t, kp_b)], (v_t, v_b)

        def mk(q0, n, i):
            return dict(pieces=[(qn_t[:, q0:q0 + n], qn_b, 0, slice(0, 128)), (qr_t[:, q0:q0 + n], qr_b, 1, slice(0, 64))],
                        n=n, O=ps_o.next(), acc=accs.next(), eng=engs[i % 4])
        for g0 in range(0, len(lat_blocks), 4):
            vqs = [mk(q0, n, i) for i, (q0, n) in enumerate(lat_blocks[g0:g0 + 4])]
            attn(vqs, kchunks, load_mla)
            for vq, (q0, n) in zip(vqs, lat_blocks[g0:g0 + 4]):
                fin_plain(vq, h, q0)
        vq = mk(ctx_blk[0], ctx_blk[1], 0)
        attn([vq], ctx_chunk, load_mla)
        fin_plain(vq, h, ctx_blk[0])

    for h in range(4):
        qn_t, qn_b = qn.next()
        k.dma("sp", qn_t[:], QB[h], writes=[qn_b])

        def load_diff(t0, nt, h=h):
            kt_t, kt_b = kbuf.next()
            v_t, v_b = vbuf.next()
            k.dma("sp", kt_t[:, 0:nt * 128], KB[h, :, t0 * 128:(t0 + nt) * 128], writes=[kt_b])
            k.dma("sp", v_t[:, 0:nt, :], VB[h, :, t0:t0 + nt, :], writes=[v_b])
            return [(kt_t, kt_b)], (v_t, v_b)

        def mkd(q0, n, t, i):
            sl = slice(64 * t, 64 * t + 64)
            return dict(pieces=[(qn_t[sl, q0:q0 + n], qn_b, 0, sl)], n=n, O=ps_o.next(), acc=accs.next(), eng=engs[i % 4])
        for g0 in range(0, len(lat_blocks), 2):
            blks = lat_blocks[g0:g0 + 2]
            vqs = []
            for i, (q0, n) in enumerate(blks):
                vqs.append(mkd(q0, n, 0, 2 * i))
                vqs.append(mkd(q0, n, 1, 2 * i + 1))
            attn(vqs, kchunks, load_diff)
            for i, (q0, n) in enumerate(blks):
                fin_diff(vqs[2 * i], vqs[2 * i + 1], 8 + h, q0)
        vqs = [mkd(ctx_blk[0], ctx_blk[1], 0, 0), mkd(ctx_blk[0], ctx_blk[1], 1, 1)]
        attn(vqs, ctx_chunk, load_diff)
        fin_diff(vqs[0], vqs[1], 8 + h, ctx_blk[0])

    kch = Rot(k, "kch", [128, HT], BF16, 1)
    vch = Rot(k, "vch", [128, HT // 128, 128], BF16, 1)
    kcc = Rot(k, "kcc", [128, CTX], BF16, 2)
    vcc = Rot(k, "vcc", [128, 2, 128], BF16, 2)
    nb = Rot(k, "nb", [128, 512], F32, 3)
    sbf = Rot(k, "sbf", [128, 512], F32, 2)
    for h in range(4):
        qn_t, qn_b = qn.next()
        k.dma("sp", qn_t[:], QC[h], writes=[qn_b])
        kh_t, kh_b = kch.next()
        vh_t, vh_b = vch.next()
        kc_t, kc_b = kcc.next()
        vc_t, vc_b = vcc.next()
        k.dma("sp", kh_t[:], KCh[h], writes=[kh_b])
        k.dma("sp", vh_t[:], VCh[h], writes=[vh_b])
        k.dma("sp", kc_t[:], KCc[h], writes=[kc_b])
        k.dma("sp", vc_t[:], VCc[h], writes=[vc_b])
        for (q0, n) in lat_blocks:
            o_t, o_b = ps_o.next()
            m_t, m_b = ps_m.next()
            for ct in range(2):
                s_t, s_b = ps_s.next()
                k.mm(s_t[:, 0:n], kc_t[:, ct * 128:(ct + 1) * 128], qn_t[:, q0:q0 + n], True, True, reads=[kc_b, qn_b], writes=[s_b])
                e_t, e_b = et.next()
                k.act(e_t[:, 0:n], s_t[:, 0:n], AF.Exp, reads=[s_b], writes=[e_b])
                k.mm(o_t[:, 0:n], vc_t[:, ct, :], e_t[:, 0:n], ct == 0, False, reads=[vc_b, e_b], writes=[o_b])
                k.mm(m_t[:, 0:n], ones_t[:], e_t[:, 0:n], ct == 0, False, reads=[ones_b, e_b], writes=[m_b])
            for rr in range(n // 64):
                r = q0 // 64 + rr
                tt0 = (r + 1) // 2
                b_t, b_b = nb.next()
                k.dma("sp", b_t[:], nabias[h, r], writes=[b_b])
                s_t, s_b = ps_s.next()
                for kk in range(8):
                    k.mm(s_t[:, kk * 64:(kk + 1) * 64], kh_t[:, (tt0 + kk) * 128:(tt0 + kk + 1) * 128],
                         qn_t[:, r * 64:(r + 1) * 64], True, True, reads=[kh_b, qn_b], writes=[s_b])
                f_t, f_b = sbf.next()
                k.tt(f_t[:], s_t[:], b_t[:], ALU.add, reads=[s_b, b_b], writes=[f_b])
                e_t, e_b = et.next()
                k.act(e_t[:], f_t[:], AF.Exp, reads=[f_b], writes=[e_b])
                for kk in range(8):
                    k.mm(o_t[:, rr * 64:(rr + 1) * 64], vh_t[:, tt0 + kk, :], e_t[:, kk * 64:(kk + 1) * 64], False, kk == 7,
                         reads=[vh_b, e_b], writes=[o_b])
                    k.mm(m_t[:, rr * 64:(rr + 1) * 64], ones_t[:], e_t[:, kk * 64:(kk + 1) * 64], False, kk == 7,
                         reads=[ones_b, e_b], writes=[m_b])
            r_t, r_b = rec.next()
            k.recip(r_t[:, 0:n], m_t[:, 0:n], reads=[m_b], writes=[r_b])
            k.tt(attnT[:, 12 + h, q0:q0 + n], o_t[:, 0:n], r_t[:, 0:n], ALU.mult, reads=[o_b, r_b], writes=[b_attnT[12 + h]])
        q0, n = ctx_blk
        o_t, o_b = ps_o.next()
        m_t, m_b = ps_m.next()
        for ct in range(2):
            s_t, s_b = ps_s.next()
            k.mm(s_t[:, 0:n], kc_t[:, ct * 128:(ct + 1) * 128], qn_t[:, q0:q0 + n], True, True, reads=[kc_b, qn_b], writes=[s_b])
            e_t, e_b = et.next()
            k.act(e_t[:, 0:n], s_t[:, 0:n], AF.Exp, reads=[s_b], writes=[e_b])
            k.mm(o_t[:, 0:n], vc_t[:, ct, :], e_t[:, 0:n], ct == 0, ct == 1, reads=[vc_b, e_b], writes=[o_b])
            k.mm(m_t[:, 0:n], ones_t[:], e_t[:, 0:n], ct == 0, ct == 1, reads=[ones_b, e_b], writes=[m_b])
        r_t, r_b = rec.next()
        k.recip(r_t[:, 0:n], m_t[:, 0:n], reads=[m_b], writes=[r_b])
        k.tt(attnT[:, 12 + h, q0:q0 + n], o_t[:, 0:n], r_t[:, 0:n], ALU.mult, reads=[o_b, r_b], writes=[b_attnT[12 + h]])

    wo = Rot(k, "wo", [128, KC, 128], BF16, 2)
    xr = Rot(k, "xr", [128, NT], F32, 1)
    for m in range(16):
        w_t, w_b = wo.next()
        k.dma("pool", w_t[:], w_out[m], writes=[w_b])
        x_t, x_b = xr.next()
        k.dma("sp", x_t[:], xT[m], writes=[x_b])
        y_t, y_b = x_t, x_b
        for (q0, n), s in [(b, 0) for b in lat_blocks] + [(ctx_blk, 1)]:
            p_t, p_b = ps_o.next()
            for kc in range(KC):
                k.mm(p_t[:, 0:n], w_t[:, kc, :], attnT[:, kc, q0:q0 + n], kc == 0, kc == KC - 1, reads=[w_b, b_attnT[kc]], writes=[p_b])
            k.stt(y_t[:, q0:q0 + n], p_t[:, 0:n], modT[:, 32 + m, s:s + 1], x_t[:, q0:q0 + n], ALU.mult, ALU.add,
                  reads=[p_b, b_modT, x_b], writes=[y_b])
        k.dma("sp", x1T[m], y_t[:], reads=[y_b], writes=[b_x1T])
    if debug:
        dbg, b_dbg = k.dout("attn_dbg", [128, 16, NT], BF16)
        k.dma("sp", dbg, attnT[:], reads=b_attnT, writes=[b_dbg])
    print("B stats", k.P.stats())
    return k.finish()


def na_bias_tiles(rpb, core, R, rows_total):
    NEG = np.float32(-30000.0)
    out = np.full((4, R, 128, 8, 64), NEG, np.float32)
    c = np.arange(64)
    wc = np.clip(c - 8, 0, 64 - 16)
    cp = np.arange(64)
    colvalid = (cp[:, None] >= wc[None, :]) & (cp[:, None] < wc[None, :] + 16)
    colrel = np.clip(cp[:, None] - c[None, :] + 15, 0, 30)
    for r in range(R):
        gr = core * R + r
        rs = min(max(gr - 4, 0), rows_total - 8)
        t0 = (r + 1) // 2
        for kk in range(8):
            for half in range(2):
                lr = 2 * (t0 + kk) + half - 8
                gk = core * R + lr
                if gk < rs or gk >= rs + 8:
                    continue
                rowrel = gk - gr + 7
                vals = rpb[:, rowrel][:, colrel]
                vals = np.where(colvalid[None], vals, NEG)
                out[:, r, half * 64:(half + 1) * 64, kk, :] = vals
    return np.ascontiguousarray(out.reshape(4, R, 128, 512))


def core_inputs_A(xT_c, inp, LA, core, NL):
    cvec = np.ascontiguousarray(np.stack([vec_fm(inp["c"][0]), vec_fm(inp["c_ctx"])], axis=-1))
    cosT, sinS = rope_tables(np.arange(core * NL, (core + 1) * NL), CTX)
    m = dict(LA)
    m.update(xT=xT_c, cvec=cvec, cosT=cosT, sinS=sinS)
    return m


def prep_B_layer(inp, l):
    lam_init = 0.8 - 0.6 * math.exp(-0.3 * l)
    lamc = np.zeros((128, 2), np.float32)
    lamc[:, 0] = lam_init
    lamc[:, 1] = 1.0 - lam_init
    return dict(
        w_out_fm=fm_tiles(inp["w_out"][l], np.arange(D)),
        dlam=np.ascontiguousarray(np.broadcast_to(inp["diff_lambda"][l].reshape(1, 256), (128, 256))),
        subln=np.ascontiguousarray(inp["diff_subln"][l].reshape(128, 1)),
        lamc=lamc,
    )


def glue_AB(resA, xT_list, inp, LB, l, NL, SEQ):
    n = len(resA)
    R = NL // 64
    rows_total = SEQ // 64
    ntl = NL // 128
    cat_k = lambda name: np.concatenate([r[name][..., :NL] for r in resA] + [resA[0][name][..., NL:]], axis=-1)
    cat_v = lambda name: np.concatenate([r[name][:, :, :ntl] for r in resA] + [resA[0][name][:, :, ntl:]], axis=2)
    shared = dict(KAn_all=cat_k("KAn"), KPE_all=cat_k("KPE"), VA_all=cat_v("VA"), KB_all=cat_k("KB"), VB_all=cat_v("VB"))
    kc_lat = np.concatenate([r["KC"][..., :NL] for r in resA], axis=-1)
    vc_lat = np.concatenate([r["VC"][:, :, :ntl] for r in resA], axis=2)
    zk = np.zeros((4, 128, 512), kc_lat.dtype)
    zv = np.zeros((4, 128, 4, 128), vc_lat.dtype)
    kc_pad = np.concatenate([zk, kc_lat, zk], axis=-1)
    vc_pad = np.concatenate([zv, vc_lat, zv], axis=2)
    maps = []
    for c in range(n):
        r = resA[c]
        m = dict(shared)
        m.update(LB)
        m.update(QAn=r["QAn"], QAr=r["QAr"], QB=r["QB"], QC=r["QC"], modT=r["modT"], xT=xT_list[c])
        m["KC_h"] = np.ascontiguousarray(kc_pad[:, :, c * NL:c * NL + NL + 1024])
        m["VC_h"] = np.ascontiguousarray(vc_pad[:, :, c * ntl:c * ntl + ntl + 8])
        m["KC_ctx"] = np.ascontiguousarray(r["KC"][..., NL:])
        m["VC_ctx"] = np.ascontiguousarray(r["VC"][:, :, ntl:])
        m["nabias"] = na_bias_tiles(inp["na_rpb"][l], c, R, rows_total)
        maps.append(m)
    return maps


NE = 32
TP = 768


def build_C(NL):
    NT = NL + CTX
    npass = NT // TP
    k = K()
    x1T = k.din("x1T", [KC, 128, NT])
    modT_d = k.din("modT", [128, 96, 2])
    ffn_norm = k.din("ffn_norm", [128, KC])
    rw_d = k.din("router_w", [128, KC, NE])
    rb_d = k.din("router_b", [128, NE])
    wgu_d = k.din("w_gu", [NE, 6, 128, KC, 256])
    bgu_d = k.din("b_gu", [128, NE, 6, 2])
    wd_d = k.din("w_down", [NE, 128, 16, 6, 128])
    bd_d = k.din("b_down", [NE, D])
    sel_d = k.din("sel", [NE, NE, 128])
    ident_d = k.din("ident", [128, 128])
    x2T, b_x2T = k.dout("x2T", [KC, 128, NT])

    masks = make_masks(k)
    modT = k.sb("modT", [128, 96, 2]); b_modT = Buf("modT")
    k.dma("sp", modT[:], modT_d, writes=[b_modT])
    fn = k.sb("fn", [128, KC]); b_fn = Buf("fn")
    k.dma("sp", fn[:], ffn_norm, writes=[b_fn])
    A2 = k.sb("A2", [128, KC, 2]); b_A2 = Buf("A2")
    for s in range(2):
        k.ts(A2[:, :, s], modT[:, 64:80, s], 1.0, math.sqrt(float(D)), ALU.add, ALU.mult, reads=[b_modT], writes=[b_A2])
        k.tt(A2[:, :, s], A2[:, :, s], fn[:], ALU.mult, reads=[b_A2, b_fn], writes=[b_A2])
    rw = k.sb("rw", [128, KC, NE], BF16); b_rw = Buf("rw")
    k.dma("pool", rw[:], rw_d, writes=[b_rw])
    rb = k.sb("rb", [128, NE]); b_rb = Buf("rb")
    k.dma("sp", rb[:], rb_d, writes=[b_rb])
    bgu = k.sb("bgu", [128, NE, 6, 2]); b_bgu = Buf("bgu")
    k.dma("sp", bgu[:], bgu_d, writes=[b_bgu])
    k.ts(bgu[:, :, :, 1], bgu[:, :, :, 1], 1.0, None, ALU.add, reads=[b_bgu], writes=[b_bgu])
    bd = k.sb("bd", [NE, D], BF16); b_bd = Buf("bd")
    k.dma("pool", bd[:], bd_d, writes=[b_bd])
    sel = k.sb("sel", [NE, NE, 128], BF16); b_sel = Buf("sel")
    k.dma("pool", sel[:], sel_d, writes=[b_sel])
    ident = k.sb("ident", [128, 128], BF16); b_ident = Buf("ident")
    k.dma("pool", ident[:], ident_d, writes=[b_ident])

    xb = k.sb("xb", [128, KC, 256]); b_xb = Buf("xb")
    sq = Rot(k, "sq", [128, 512], BF16, 3)
    tmpf = Rot(k, "tmpf", [128, 256], F32, 2)
    sqs = Rot(k, "sqs", [128, 256], F32, 1)
    rstd = Rot(k, "rstd", [128, 256], F32, 2)
    h2T = k.sb("h2T", [128, KC, TP], BF16); b_h2T = [Buf(f"h2T{c}") for c in range(KC)]
    GT = k.sb("GT", [NE, TP], BF16); b_GT = Buf("GT")
    gbc = k.sb("gbc", [128, TP]); b_gbc = Buf("gbc")
    yacc = k.sb("yacc", [128, 16, TP]); b_yacc = [Buf(f"yacc{m}") for m in range(16)]
    actT = k.sb("actT", [128, 6, TP], BF16); b_actT = [Buf(f"actT{j}") for j in range(6)]
    wgu = Rot(k, "wgu", [128, KC, 256], BF16, 2)
    wd = Rot(k, "wd", [128, 8, 6, 128], BF16, 3)
    lg = Rot(k, "lg", [128, NE], F32, 2)
    m8 = Rot(k, "m8", [128, 8], F32, 2)
    sm = Rot(k, "smc", [128, 4], F32, 2)
    ex = Rot(k, "ex", [128, NE], F32, 2)
    gb16 = Rot(k, "gb16", [128, NE], BF16, 2)
    gt_ = Rot(k, "g_", [128, 512], F32, 2)
    sg_ = Rot(k, "sg_", [128, 512], F32, 2)
    l1_ = Rot(k, "l1_", [128, 512], F32, 2)
    gg_ = Rot(k, "gg_", [128, 512], F32, 2)
    xr = Rot(k, "xr", [128, TP], F32, 1)
    pp = Rot(k, "pp", [128, 512], F32, 4, psum=True)
    pss = Rot(k, "pss", [128, 512], F32, 1, psum=True)
    psg = Rot(k, "psg", [128, 512], F32, 2, psum=True)
    ones_t, ones_b = masks["ones"]

    for p in range(npass):
        pblocks = [(p * TP, 0, 512), (p * TP + 512, 512, 256)]
        for (g0, l0, n) in [(p * TP + i * 256, i * 256, 256) for i in range(3)]:
            s = 1 if g0 >= NL else 0
            k.dma("sp", xb[:, :, 0:n], x1T[:, :, g0:g0 + n].rearrange("c p t -> p c t"), writes=[b_xb])
            pt, pb = pss.next()
            for c in range(KC):
                st_, sb_ = sq.next()
                k.act(st_[:, 0:n], xb[:, c, 0:n], AF.Square, reads=[b_xb], writes=[sb_])
                k.mm(pt[:, 0:n], ones_t[:], st_[:, 0:n], c == 0, c == KC - 1, reads=[ones_b, sb_], writes=[pb])
            s_t, s_b = sqs.next()
            k.act(s_t[:, 0:n], pt[:, 0:n], AF.Sqrt, reads=[pb], writes=[s_b], bias=float(D * EPS))
            r_t, r_b = rstd.next()
            k.recip(r_t[:, 0:n], s_t[:, 0:n], reads=[s_b], writes=[r_b])
            for c in range(KC):
                t1, t1b = tmpf.next()
                k.stt(t1[:, 0:n], xb[:, c, 0:n], A2[:, c, s:s + 1], r_t[:, 0:n], ALU.mult, ALU.mult,
                      reads=[b_xb, b_A2, r_b], writes=[t1b])
                k.act(h2T[:, c, l0:l0 + n], t1[:, 0:n], AF.Identity, reads=[t1b, b_modT], writes=[b_h2T[c]],
                      bias=modT[:, 48 + c, s:s + 1])
        for tt in range(TP // 128):
            pt, pb = pp.next()
            for c in range(KC):
                k.mm(pt[:, 0:NE], h2T[:, c, tt * 128:(tt + 1) * 128], rw[:, c, :], c == 0, c == KC - 1,
                     reads=[b_h2T[c], b_rw], writes=[pb])
            l_t, l_b = lg.next()
            k.tt(l_t[:], pt[:, 0:NE], rb[:], ALU.add, reads=[pb, b_rb], writes=[l_b])
            m_t, m_b = m8.next()
            k.P.op("dve", lambda e, m_t=m_t, l_t=l_t: e.max(out=m_t[:], in_=l_t[:]), reads=[l_b], writes=[m_b])
            c_t, c_b = sm.next()
            k.ts(c_t[:, 0:1], m_t[:, 0:1], -1.0, None, ALU.mult, reads=[m_b], writes=[c_b])
            e_t, e_b = ex.next()
            k.act(e_t[:], l_t[:], AF.Exp, reads=[l_b, c_b], writes=[e_b], bias=c_t[:, 0:1])
            k.stt(e_t[:], l_t[:], m_t[:, 3:4], e_t[:], ALU.is_ge, ALU.mult, reads=[l_b, m_b, e_b], writes=[e_b])
            k.P.op("dve", lambda e, c_t=c_t, e_t=e_t: e.reduce_sum(out=c_t[:, 1:2], in_=e_t[:], axis=mybir.AxisListType.X),
                   reads=[e_b], writes=[c_b])
            k.recip(c_t[:, 2:3], c_t[:, 1:2], reads=[c_b], writes=[c_b])
            g_t, g_b = gb16.next()
            k.ts(g_t[:], e_t[:], c_t[:, 2:3], None, ALU.mult, reads=[e_b, c_b], writes=[g_b])
            tp_t, tp_b = psg.next()
            k.mm(tp_t[0:NE, 0:128], g_t[:], ident[:], True, True, reads=[g_b, b_ident], writes=[tp_b])
            k.copy(GT[:, tt * 128:(tt + 1) * 128], tp_t[0:NE, 0:128], reads=[tp_b], writes=[b_GT])
        blocks = [(0, 512), (512, 256)]
        for e in range(NE):
            wds = []
            for hf in range(2):
                wd_t, wd_b = wd.next()
                k.dma("pool", wd_t[:], wd_d[e, :, hf * 8:hf * 8 + 8], writes=[wd_b])
                wds.append((wd_t, wd_b))
            for (l0, n) in blocks:
                g_t, g_b = psg.next()
                k.mm(g_t[:, 0:n], sel[:, e, :], GT[:, l0:l0 + n], True, True, reads=[b_sel, b_GT], writes=[g_b])
                k.act(gbc[:, l0:l0 + n], g_t[:, 0:n], AF.Copy, reads=[g_b], writes=[b_gbc])
            for j in range(6):
                w_t, w_b = wgu.next()
                k.dma("pool", w_t[:], wgu_d[e, j], writes=[w_b])
                for (l0, n) in blocks:
                    pg, pgb = pp.next()
                    for c in range(KC):
                        k.mm(pg[:, 0:n], w_t[:, c, 0:128], h2T[:, c, l0:l0 + n], c == 0, c == KC - 1, reads=[w_b, b_h2T[c]], writes=[pgb])
                    pl, plb = pp.next()
                    for c in range(KC):
                        k.mm(pl[:, 0:n], w_t[:, c, 128:256], h2T[:, c, l0:l0 + n], c == 0, c == KC - 1, reads=[w_b, b_h2T[c]], writes=[plb])
                    a_t, a_b = gt_.next()
                    k.ts(a_t[:, 0:n], pg[:, 0:n], bgu[:, e, j, 0:1], 7.0, ALU.add, ALU.min, reads=[pgb, b_bgu], writes=[a_b])
                    s_t, s_b = sg_.next()
                    k.act(s_t[:, 0:n], a_t[:, 0:n], AF.Sigmoid, reads=[a_b], writes=[s_b], scale=1.702)
                    l_t, l_b = l1_.next()
                    k.ts(l_t[:, 0:n], pl[:, 0:n], bgu[:, e, j, 1:2], 8.0, ALU.add, ALU.min, reads=[plb, b_bgu], writes=[l_b])
                    q_t, q_b = gg_.next()
                    k.tt(q_t[:, 0:n], a_t[:, 0:n], gbc[:, l0:l0 + n], ALU.mult, reads=[a_b, b_gbc], writes=[q_b], eng="pool")
                    u_t, u_b = q_t, q_b
                    k.tt(u_t[:, 0:n], q_t[:, 0:n], s_t[:, 0:n], ALU.mult, reads=[q_b, s_b], writes=[u_b], eng="pool")
                    k.stt(actT[:, j, l0:l0 + n], l_t[:, 0:n], -6.0, u_t[:, 0:n], ALU.max, ALU.mult,
                          reads=[l_b, u_b], writes=[b_actT[j]])
            for m in range(16):
                wd_t, wd_b = wds[m // 8]
                for (l0, n) in blocks:
                    py, pyb = pp.next()
                    for j in range(6):
                        k.mm(py[:, 0:n], wd_t[:, m % 8, j, :], actT[:, j, l0:l0 + n], j == 0, (j == 5 and e != 0),
                             reads=[wd_b, b_actT[j]], writes=[pyb])
                    if e == 0:
                        k.mm(py[:, 0:n], bd[:, m * 128:(m + 1) * 128], GT[:, l0:l0 + n], False, True, reads=[b_bd, b_GT], writes=[pyb])
                        k.copy(yacc[:, m, l0:l0 + n], py[:, 0:n], reads=[pyb], writes=[b_yacc[m]])
                    else:
                        k.tt(yacc[:, m, l0:l0 + n], yacc[:, m, l0:l0 + n], py[:, 0:n], ALU.add, reads=[b_yacc[m], pyb], writes=[b_yacc[m]])
        for m in range(16):
            x_t, x_b = xr.next()
            k.dma("sp", x_t[:], x1T[m, :, p * TP:(p + 1) * TP], writes=[x_b])
            y_t, y_b = x_t, x_b
            for (g0, l0, n) in pblocks:
                s = 1 if g0 >= NL else 0
                k.stt(y_t[:, l0:l0 + n], yacc[:, m, l0:l0 + n], modT[:, 80 + m, s:s + 1], x_t[:, l0:l0 + n], ALU.mult, ALU.add,
                      reads=[b_yacc[m], b_modT, x_b], writes=[y_b])
            k.dma("sp", x2T[m, :, p * TP:(p + 1) * TP], y_t[:], reads=[y_b], writes=[b_x2T])
    print("C stats", k.P.stats())
    return k.finish()


def prep_C_layer(inp, l):
    w_gu = inp["w_gu"][l]
    wg = w_gu.reshape(NE, KC, 128, 6, 128, 2).transpose(0, 3, 2, 1, 5, 4).reshape(NE, 6, 128, KC, 256)
    b_gu = inp["b_gu"][l].reshape(NE, 6, 128, 2).transpose(2, 0, 1, 3)
    w_dn = inp["w_down"][l].reshape(NE, 6, 128, 16, 128).transpose(0, 2, 3, 1, 4)
    sel = np.zeros((NE, NE, 128), np.float32)
    for e in range(NE):
        sel[e, e, :] = 1.0
    return dict(
        ffn_norm=vec_fm(inp["ffn_norm"][l]),
        router_w=tm_tiles(inp["router_w"][l], np.arange(NE)),
        router_b=np.ascontiguousarray(np.broadcast_to(inp["router_b"][l].reshape(1, NE), (128, NE))),
        w_gu=np.ascontiguousarray(wg),
        b_gu=np.ascontiguousarray(b_gu),
        w_down=np.ascontiguousarray(w_dn),
        b_down=np.ascontiguousarray(inp["b_down"][l]),
        sel=sel,
        ident=np.eye(128, dtype=np.float32),
    )


_PROGS = {}


def _prog(name, fn, *args):
    key = (name,) + args
    if key not in _PROGS:
        _PROGS[key] = fn(*args)
    return _PROGS[key]


def kernel(**inputs):
    inp = {k_: np.asarray(v) for k_, v in inputs.items()}
    SEQ = inp["x"].shape[1]
    depth = inp["w_in"].shape[0]
    NL = SEQ // NCORES
    NT = NL + CTX
    cores = list(range(NCORES))
    xT_list = []
    for c in cores:
        xs = np.concatenate([inp["x"][0, c * NL:(c + 1) * NL], inp["ctx"][0]], axis=0)
        xT_list.append(np.ascontiguousarray(xs.T.reshape(KC, 128, NT)))
    for l in range(depth):
        ncA = _prog("A", build_A, NL)
        LA = prep_A_layer(inp, l)
        resA = run_bass_kernel_spmd(ncA, [core_inputs_A(xT_list[c], inp, LA, c, NL) for c in cores], core_ids=cores).results
        del LA
        ncB = _prog("B", build_B, NL, SEQ)
        LB = prep_B_layer(inp, l)
        mapsB = glue_AB(resA, xT_list, inp, LB, l, NL, SEQ)
        resB = run_bass_kernel_spmd(ncB, mapsB, core_ids=cores).results
        del mapsB
        ncC = _prog("C", build_C, NL)
        LC = prep_C_layer(inp, l)
        mapsC = []
        for c in cores:
            m = dict(LC)
            m.update(x1T=resB[c]["x1T"], modT=resA[c]["modT"])
            mapsC.append(m)
        resC = run_bass_kernel_spmd(ncC, mapsC, core_ids=cores).results
        del mapsC, LC
        xT_list = [np.ascontiguousarray(resC[c]["x2T"]) for c in cores]
    out = np.concatenate([xT_list[c].reshape(D, NT)[:, :NL].T for c in cores], axis=0)[None]
    return np.ascontiguousarray(out.astype(np.float32))
```
